# Optimizing a Trainium2 kernel written in Bass

```python
import math
import jax
import jax.numpy as jnp
from jax import lax
import numpy as np


D_MODEL = 4096
BATCH = 2
SEQ = 8192
DEPTH = 2

N_BRANCH = 4
D_BRANCH = D_MODEL // 4
EPS = 1e-6
LRU_BLOCKS = 8
LRU_BLOCK = D_BRANCH // LRU_BLOCKS
LRU_CONV = 4
LRU_C = 8.0
POOL_WINDOWS = (2, 4, 8, 16)
POOL_GROUP = D_BRANCH // len(POOL_WINDOWS)
DA_HEAD_DIM = 64
DA_HEADS = D_BRANCH // (2 * DA_HEAD_DIM)
DA_V_DIM = 2 * DA_HEAD_DIM
ATTN_SCALE = 1.0 / math.sqrt(DA_HEAD_DIM)
ROPE_DIM = DA_HEAD_DIM // 4
ROPE_THETA = 500000.0
Q_BLOCK = 128
CV_KERNEL = 31
N_GROUPS = 4
EXPERTS_PER_GROUP = 8
N_EXPERTS = N_GROUPS * EXPERTS_PER_GROUP
TOP_K = 2
D_EXPERT = 512
MAX_POS_OFFSET = 4096
OFF_LRU_X = 0
OFF_LRU_GATE = D_BRANCH
OFF_POOL = 2 * D_BRANCH
OFF_Q = 3 * D_BRANCH
OFF_K = 4 * D_BRANCH
OFF_V = 5 * D_BRANCH
OFF_GLU = 6 * D_BRANCH
N_MIX = 8 * D_BRANCH
N_IN = N_MIX + N_BRANCH * D_MODEL

kernel_name = 'hybrid_rglru_pool_diffattn_conformer_hiermoe'


def _rmsnorm(x, g):
    xf = x.astype(jnp.float32)
    y = xf * lax.rsqrt(jnp.mean(xf * xf, axis=-1, keepdims=True) + EPS)
    return (y * g.astype(jnp.float32)).astype(x.dtype)


def _layernorm(x, g, b):
    xf = x.astype(jnp.float32)
    mu = jnp.mean(xf, axis=-1, keepdims=True)
    var = jnp.mean(jnp.square(xf - mu), axis=-1, keepdims=True)
    y = (xf - mu) * lax.rsqrt(var + EPS)
    return (y * g.astype(jnp.float32) + b.astype(jnp.float32)).astype(x.dtype)


def _causal_dwconv(x, w, b):
    width, ch = w.shape
    y = lax.conv_general_dilated(x, w[:, None, :].astype(x.dtype), window_strides=(1,),
                                 padding=[(width - 1, 0)],
                                 dimension_numbers=('NWC', 'WIO', 'NWC'),
                                 feature_group_count=ch)
    return y + b.astype(x.dtype)


def _partial_rope(x, cos, sin):
    half = ROPE_DIM // 2
    x1 = x[..., :half].astype(jnp.float32)
    x2 = x[..., half:ROPE_DIM].astype(jnp.float32)
    rot = jnp.concatenate([x1 * cos - x2 * sin, x2 * cos + x1 * sin], axis=-1).astype(x.dtype)
    return jnp.concatenate([rot, x[..., ROPE_DIM:]], axis=-1)


def _rg_lru(xa, w_r, b_r, w_i, b_i, lam):
    bsz, s, _ = xa.shape
    xf = xa.astype(jnp.float32)
    xh = xf.reshape(bsz, s, LRU_BLOCKS, LRU_BLOCK)
    r = jax.nn.sigmoid(jnp.einsum('bshi,hij->bshj', xh, w_r.astype(jnp.float32)).reshape(bsz, s, D_BRANCH) + b_r.astype(jnp.float32))
    i = jax.nn.sigmoid(jnp.einsum('bshi,hij->bshj', xh, w_i.astype(jnp.float32)).reshape(bsz, s, D_BRANCH) + b_i.astype(jnp.float32))
    log_a = -LRU_C * r * jax.nn.softplus(-lam.astype(jnp.float32))
    a = jnp.exp(log_a)
    u = jnp.sqrt(-jnp.expm1(2.0 * log_a)) * (i * xf)

    def combine(left, right):
        a_l, u_l = left
        a_r, u_r = right
        return a_l * a_r, a_r * u_l + u_r

    _, h = lax.associative_scan(combine, (a, u), axis=1)
    return h.astype(xa.dtype)


def _pool_mixer(xp, w_pool, b_pool, scale):
    bsz, s, _ = xp.shape
    xf = xp.astype(jnp.float32)
    csum = jnp.pad(jnp.cumsum(xf, axis=1), ((0, 0), (1, 0), (0, 0)))
    t1 = jnp.arange(1, s + 1, dtype=jnp.float32)[None, :, None]
    diffs = []
    for gi, win in enumerate(POOL_WINDOWS):
        sl = slice(gi * POOL_GROUP, (gi + 1) * POOL_GROUP)
        cg = csum[..., sl]
        upper = cg[:, 1:]
        lower = jnp.pad(cg, ((0, 0), (win - 1, 0), (0, 0)))[:, :s]
        mean = (upper - lower) / jnp.minimum(t1, float(win))
        diffs.append(mean - xf[..., sl])
    d = jnp.stack(diffs, axis=2)
    y = jnp.einsum('bsgi,gij->bsgj', d, w_pool.astype(jnp.float32)) + b_pool.astype(jnp.float32).reshape(len(POOL_WINDOWS), POOL_GROUP)
    return (y.reshape(bsz, s, D_BRANCH) * scale.astype(jnp.float32)).astype(xp.dtype)


def _diff_attention(q, k, v, cos, sin, q_g, k_g, lq1, lk1, lq2, lk2, sub_g, lambda_init):
    bsz, s = q.shape[:2]
    q = _partial_rope(_rmsnorm(q, q_g), cos, sin)
    k = _partial_rope(_rmsnorm(k, k_g), cos, sin)
    f32 = jnp.float32
    lam = (jnp.exp(jnp.sum(lq1.astype(f32) * lk1.astype(f32)))
           - jnp.exp(jnp.sum(lq2.astype(f32) * lk2.astype(f32))) + lambda_init)
    n_blk = s // Q_BLOCK
    qb = q.reshape(bsz, n_blk, Q_BLOCK, DA_HEADS, 2, DA_HEAD_DIM).transpose(1, 0, 2, 3, 4, 5)
    kpos = jnp.arange(s)

    def block(args):
        q_blk, blk = args
        sc = jnp.einsum('bqhcd,bkhcd->bhcqk', q_blk, k, preferred_element_type=f32) * ATTN_SCALE
        qpos = blk * Q_BLOCK + jnp.arange(Q_BLOCK)
        sc = jnp.where(kpos[None, :] <= qpos[:, None], sc, -jnp.inf)
        p = jax.nn.softmax(sc, axis=-1)
        amap = p[:, :, 0] - lam * p[:, :, 1]
        return jnp.einsum('bhqk,bkhd->bqhd', amap.astype(v.dtype), v)

    o = lax.map(block, (qb, jnp.arange(n_blk)))
    o = o.transpose(1, 0, 2, 3, 4).reshape(bsz, s, DA_HEADS, DA_V_DIM)
    o = _rmsnorm(o, sub_g) * (1.0 - lambda_init)
    return o.reshape(bsz, s, D_BRANCH)


def _conformer_conv(xg, dw_w, dw_b, ln_g, ln_b):
    val, gt = jnp.split(xg, 2, axis=-1)
    u = val * jax.nn.sigmoid(gt)
    u = _causal_dwconv(u, dw_w, dw_b)
    return jax.nn.silu(_layernorm(u, ln_g, ln_b))


def _hier_moe(h, wg, bg, we, be, w1, w3, w2):
    bsz, s, d = h.shape
    tok = h.reshape(bsz * s, d)
    g_prob = jax.nn.softmax((tok @ wg + bg).astype(jnp.float32), axis=-1)
    g_top, g_idx = lax.top_k(g_prob, 1)
    e_logits = (tok @ we + be).astype(jnp.float32).reshape(-1, N_GROUPS, EXPERTS_PER_GROUP)
    e_in = jnp.take_along_axis(e_logits, g_idx[:, :, None], axis=1)[:, 0]
    e_val, e_idx = lax.top_k(e_in, TOP_K)
    w_tok = g_top * jax.nn.softmax(e_val, axis=-1)
    expert_id = g_idx * EXPERTS_PER_GROUP + e_idx
    comb = jnp.einsum('nk,nke->ne', w_tok, jax.nn.one_hot(expert_id, N_EXPERTS, dtype=jnp.float32)).astype(h.dtype)
    out = jnp.zeros_like(tok)
    for e in range(N_EXPERTS):
        hid = jax.nn.silu(tok @ w1[e]) * (tok @ w3[e])
        out = out + comb[:, e:e + 1] * (hid @ w2[e])
    return out.reshape(bsz, s, d)


def setup_inputs(seed: int = 0) -> dict:
    key = jax.random.key(seed)
    ks = jax.random.split(key, 40)
    L = DEPTH
    f32 = jnp.float32

    def nrm(i, shape, scale):
        return jax.random.normal(ks[i], shape, f32) * scale

    def gain(i, shape):
        return 1.0 + 0.1 * jax.random.normal(ks[i], shape, f32)

    x = nrm(0, (BATCH, SEQ, D_MODEL), 1.0)
    c = nrm(1, (BATCH, D_MODEL), 1.0)
    positions = (jax.random.randint(ks[2], (BATCH, 1), 0, MAX_POS_OFFSET, jnp.int32)
                 + jnp.arange(SEQ, dtype=jnp.int32)[None, :])
    a0 = jax.random.uniform(ks[16], (L, D_BRANCH), f32, minval=0.9, maxval=0.999)
    return {
        'x': x,
        'c': c,
        'positions': positions,
        'ada_w': nrm(3, (L, D_MODEL, 6 * D_MODEL), 0.5 * D_MODEL ** -0.5),
        'ada_b': nrm(4, (L, 6 * D_MODEL), 0.02),
        'norm1_g': gain(5, (L, D_MODEL)),
        'norm2_g': gain(6, (L, D_MODEL)),
        'w_in': nrm(7, (L, D_MODEL, N_IN), D_MODEL ** -0.5),
        'b_gate': nrm(8, (L, N_BRANCH, D_MODEL), 0.1),
        'lru_conv_w': nrm(9, (L, LRU_CONV, D_BRANCH), LRU_CONV ** -0.5),
        'lru_conv_b': nrm(10, (L, D_BRANCH), 0.02),
        'lru_wr': nrm(11, (L, LRU_BLOCKS, LRU_BLOCK, LRU_BLOCK), LRU_BLOCK ** -0.5),
        'lru_br': nrm(12, (L, D_BRANCH), 0.1),
        'lru_wi': nrm(13, (L, LRU_BLOCKS, LRU_BLOCK, LRU_BLOCK), LRU_BLOCK ** -0.5),
        'lru_bi': nrm(14, (L, D_BRANCH), 0.1),
        'lru_lambda': jnp.log(a0) - jnp.log1p(-a0),
        'pool_w': nrm(17, (L, len(POOL_WINDOWS), POOL_GROUP, POOL_GROUP), POOL_GROUP ** -0.5),
        'pool_b': nrm(18, (L, D_BRANCH), 0.02),
        'pool_scale': gain(19, (L, D_BRANCH)),
        'q_norm_g': gain(20, (L, DA_HEAD_DIM)),
        'k_norm_g': gain(21, (L, DA_HEAD_DIM)),
        'lam_q1': nrm(22, (L, DA_HEAD_DIM), 0.1),
        'lam_k1': nrm(23, (L, DA_HEAD_DIM), 0.1),
        'lam_q2': nrm(24, (L, DA_HEAD_DIM), 0.1),
        'lam_k2': nrm(25, (L, DA_HEAD_DIM), 0.1),
        'subln_g': gain(26, (L, DA_V_DIM)),
        'cv_dw_w': nrm(27, (L, CV_KERNEL, D_BRANCH), CV_KERNEL ** -0.5),
        'cv_dw_b': nrm(28, (L, D_BRANCH), 0.02),
        'cv_ln_g': gain(29, (L, D_BRANCH)),
        'cv_ln_b': nrm(30, (L, D_BRANCH), 0.02),
        'w_branch': nrm(31, (L, N_BRANCH, D_BRANCH, D_MODEL), D_BRANCH ** -0.5),
        'w_out': nrm(32, (L, D_MODEL, D_MODEL), D_MODEL ** -0.5),
        'router_g_w': nrm(33, (L, D_MODEL, N_GROUPS), D_MODEL ** -0.5),
        'router_g_b': nrm(34, (L, N_GROUPS), 0.01),
        'router_e_w': nrm(35, (L, D_MODEL, N_EXPERTS), D_MODEL ** -0.5),
        'router_e_b': nrm(36, (L, N_EXPERTS), 0.01),
        'moe_w1': nrm(37, (L, N_EXPERTS, D_MODEL, D_EXPERT), D_MODEL ** -0.5),
        'moe_w3': nrm(38, (L, N_EXPERTS, D_MODEL, D_EXPERT), D_MODEL ** -0.5),
        'moe_w2': nrm(39, (L, N_EXPERTS, D_EXPERT, D_MODEL), D_EXPERT ** -0.5),
    }


def reference(x, c, positions, ada_w, ada_b, norm1_g, norm2_g, w_in, b_gate,
              lru_conv_w, lru_conv_b, lru_wr, lru_br, lru_wi, lru_bi, lru_lambda,
              pool_w, pool_b, pool_scale, q_norm_g, k_norm_g, lam_q1, lam_k1, lam_q2, lam_k2,
              subln_g, cv_dw_w, cv_dw_b, cv_ln_g, cv_ln_b, w_branch, w_out,
              router_g_w, router_g_b, router_e_w, router_e_b, moe_w1, moe_w3, moe_w2):
    bsz, s, _ = x.shape
    inv_freq = ROPE_THETA ** (-jnp.arange(0, ROPE_DIM, 2, dtype=jnp.float32) / ROPE_DIM)
    ang = positions.astype(jnp.float32)[..., None] * inv_freq
    cos = jnp.cos(ang)[:, :, None, None, :]
    sin = jnp.sin(ang)[:, :, None, None, :]
    c_act = jax.nn.silu(c)
    for l in range(DEPTH):
        mod = c_act @ ada_w[l] + ada_b[l]
        sh1, sc1, g1, sh2, sc2, g2 = jnp.split(mod[:, None, :], 6, axis=-1)
        h = _rmsnorm(x, norm1_g[l]) * (1.0 + sc1) + sh1
        wl = w_in[l]
        proj = h @ wl[:, :N_MIX]
        xa = _causal_dwconv(proj[..., OFF_LRU_X:OFF_LRU_GATE], lru_conv_w[l], lru_conv_b[l])
        y_a = jax.nn.gelu(proj[..., OFF_LRU_GATE:OFF_POOL]) * _rg_lru(
            xa, lru_wr[l], lru_br[l], lru_wi[l], lru_bi[l], lru_lambda[l])
        y_b = _pool_mixer(proj[..., OFF_POOL:OFF_Q], pool_w[l], pool_b[l], pool_scale[l])
        q = proj[..., OFF_Q:OFF_K].reshape(bsz, s, DA_HEADS, 2, DA_HEAD_DIM)
        k = proj[..., OFF_K:OFF_V].reshape(bsz, s, DA_HEADS, 2, DA_HEAD_DIM)
        v = proj[..., OFF_V:OFF_GLU].reshape(bsz, s, DA_HEADS, DA_V_DIM)
        lambda_init = 0.8 - 0.6 * math.exp(-0.3 * l)
        y_c = _diff_attention(q, k, v, cos, sin, q_norm_g[l], k_norm_g[l], lam_q1[l], lam_k1[l],
                              lam_q2[l], lam_k2[l], subln_g[l], lambda_init)
        y_d = _conformer_conv(proj[..., OFF_GLU:N_MIX], cv_dw_w[l], cv_dw_b[l], cv_ln_g[l], cv_ln_b[l])
        merged = jnp.zeros_like(x)
        for bi, y in enumerate((y_a, y_b, y_c, y_d)):
            lo = N_MIX + bi * D_MODEL
            gate = jax.nn.sigmoid(h @ wl[:, lo:lo + D_MODEL] + b_gate[l, bi])
            merged = merged + gate * (y @ w_branch[l, bi])
        x = x + g1 * (merged @ w_out[l])
        h2 = _rmsnorm(x, norm2_g[l]) * (1.0 + sc2) + sh2
        x = x + g2 * _hier_moe(h2, router_g_w[l], router_g_b[l], router_e_w[l], router_e_b[l],
                               moe_w1[l], moe_w3[l], moe_w2[l])
    return x
```

```python
import math
import numpy as np
from contextlib import ExitStack
import ml_dtypes
import concourse.bass as bass
import concourse.mybir as mybir
from concourse.bass_utils import run_bass_kernel_spmd

F32 = mybir.dt.float32
BF16 = mybir.dt.bfloat16
I32 = mybir.dt.int32
AF = mybir.ActivationFunctionType
ALU = mybir.AluOpType
AX = mybir.AxisListType

NCORES = 8
D = 4096
B = 2
SEQ = 8192
DB = 1024
NMIX = 8192
EPS = 1e-6
TPC = 2048


class T:
    __slots__ = ("ap", "name", "w", "r", "dsem", "dval", "psum", "uid")
    _n = [0]

    def __init__(self, ap, name, psum=False):
        T._n[0] += 1
        self.uid = T._n[0]
        self.ap = ap
        self.name = name
        self.psum = psum
        self.w = None
        self.r = {}
        self.dsem = None
        self.dval = 0

    def __getitem__(self, idx):
        return self.ap[idx]


class Sched:
    ENG = ("pe", "act", "dve", "pool", "sp")

    def __init__(self, nc, stack):
        self.nc = nc
        self.stack = stack
        self.eng = {"pe": nc.tensor, "act": nc.scalar, "dve": nc.vector,
                    "pool": nc.gpsimd, "sp": nc.sync}
        self.sem = {}
        self.cnt = {}
        for q in self.ENG:
            self.sem[q] = stack.enter_context(nc.semaphore("s_" + q))
            self.cnt[q] = 0
        self.seen = {q: {} for q in self.ENG}
        self.nsem = len(self.ENG)
        self.semstack = stack
        self.dtiles = {}
        self.dtot = {}

    def sb(self, name, shape, dt):
        h = self.stack.enter_context(self.nc.sbuf_tensor(name, list(shape), dt))
        return T(h, name)

    def sbn(self, name, shape, dt, n):
        full = [shape[0], n] + list(shape[1:])
        h = self.stack.enter_context(self.nc.sbuf_tensor(name, full, dt))
        return [T(h[:, i], "%s_%d" % (name, i)) for i in range(n)]

    def ps(self, name, shape, dt=F32):
        h = self.stack.enter_context(self.nc.psum_tensor(name, list(shape), dt))
        return T(h, name, psum=True)

    def _dsem(self, t):
        if t.dsem is None:
            sem = self.semstack.enter_context(self.nc.semaphore("d_" + t.name))
            self.nsem += 1
            key = ("d", t.uid)
            self.sem[key] = sem
            self.dtiles[key] = t
            t.dsem = [sem, key]
            self.dtot[key] = 0
        return t.dsem[1]

    def share_dsem(self, tiles):
        key = self._dsem(tiles[0])
        for t in tiles[1:]:
            t.dsem = tiles[0].dsem
        return key

    def _wait(self, q, deps):
        e = self.eng[q]
        seen = self.seen[q]
        for key, val in deps.items():
            if seen.get(key, 0) >= val:
                continue
            e.wait_ge(self.sem[key], val)
            seen[key] = val

    def _deps(self, q, r, w, is_dma=False):
        deps = {}

        def add(k, v):
            if isinstance(k, tuple):
                v = self.dtot[k]
            if deps.get(k, 0) < v:
                deps[k] = v
        for t in r:
            if t.w is not None:
                add(*t.w)
            if t.psum:
                for k, v in t.r.items():
                    if k != q:
                        add(k, v)
        for t in w:
            if t.w is not None:
                k = t.w[0]
                if not ((q == "pe" and k == "pe") or
                        (is_dma and t.dsem is not None and k == t.dsem[1])):
                    add(*t.w)
            for k, v in t.r.items():
                add(k, v)
        return deps

    def op(self, q, fn, r=(), w=(), inc=True):
        self._wait(q, self._deps(q, r, w))
        ins = fn(self.eng[q])
        if inc:
            self.cnt[q] += 1
            ins.then_inc(self.sem[q], 1)
            val = self.cnt[q]
        else:
            val = self.cnt[q] + 1
        for t in r:
            if t.r.get(q, 0) < val:
                t.r[q] = val
        for t in w:
            t.w = (q, val)
            t.r = {}
        return ins

    def dma(self, q, out, in_, r=(), w=(), **kw):
        tl = (list(w) + list(r))[0]
        key = self._dsem(tl)
        self._wait(q, self._deps(q, r, w, is_dma=True))
        ins = self.eng[q].dma_start(out=out, in_=in_, **kw)
        self.dtot[key] += 16
        val = self.dtot[key]
        ins.then_inc(tl.dsem[0], 16)
        for t in r:
            if t.r.get(key, 0) < val:
                t.r[key] = val
        for t in w:
            t.w = (key, val)
            t.r = {}
        return ins

    def finish(self, tiles, q="sp"):
        deps = {}
        for t in tiles:
            toks = ([t.w] if t.w is not None else []) + list(t.r.items())
            for k, v in toks:
                if isinstance(k, tuple):
                    v = self.dtot[k]
                if deps.get(k, 0) < v:
                    deps[k] = v
        self._wait(q, deps)


def _din(nc, name, shape, dt=F32):
    return nc.dram_tensor(name, list(shape), dt, kind="ExternalInput").ap()


def _dout(nc, name, shape, dt=F32):
    return nc.dram_tensor(name, list(shape), dt, kind="ExternalOutput").ap()


def build_L0():
    nc = bass.Bass("TRN2", target_bir_lowering=False)
    c = _din(nc, "c", [2, D])
    adaw = _din(nc, "adaw", [2, D, 3072])
    adab = _din(nc, "adab", [2, 1, 3072])
    ident = _din(nc, "ident", [128, 128])
    out = _dout(nc, "modT", [2, 128, 24, 2])
    with ExitStack() as st:
        S = Sched(nc, st)
        idt = S.sb("idt", [128, 128], F32)
        ct = S.sb("ct", [2, D], F32)
        ca = S.sb("ca", [2, D], F32)
        cT = S.sb("cT", [128, 32, 2], F32)
        ones2 = S.sb("ones2", [1, 2], F32)
        bt = [S.sb("bt%d" % i, [1, 3072], F32) for i in range(2)]
        wb = [S.sb("wb%d" % i, [128, 32, 512], F32) for i in range(2)]
        mo = S.sb("mo", [128, 2, 24, 2], F32)
        pT = S.ps("pT", [128, 512])
        pm = [S.ps("pm%d" % i, [128, 512]) for i in range(2)]
        S.dma("sp", idt[:], ident, w=[idt])
        S.dma("sp", ct[:], c, w=[ct])
        S.op("dve", lambda e: e.memset(ones2[:], 1.0), w=[ones2])
        S.op("act", lambda e: e.activation(out=ca[:], in_=ct[:], func=AF.Silu), r=[ct], w=[ca])
        for kc in range(32):
            S.op("pe", lambda e: e.transpose(out=pT[:, 2 * kc:2 * kc + 2], in_=ca[0:2, kc * 128:(kc + 1) * 128],
                                             identity=idt[0:2, 0:2]), r=[ca, idt], w=[pT], inc=(kc == 31))
        S.op("dve", lambda e: e.tensor_copy(out=cT[:].rearrange("p a b -> p (a b)"), in_=pT[:, 0:64]), r=[pT], w=[cT])
        n = 0
        for l in range(2):
            S.dma("sp", bt[l][:], adab[l], w=[bt[l]])
            wv = adaw[l].rearrange("(kc p) n -> p kc n", p=128)
            for grp in range(6):
                wt = wb[n % 2]
                for k0 in range(0, 32, 4):
                    S.dma("sp", wt[:, k0:k0 + 4, :], wv[:, k0:k0 + 4, grp * 512:(grp + 1) * 512], w=[wt])
                for j in range(4):
                    fc = grp * 4 + j
                    p = pm[fc % 2]
                    for kc in range(32):
                        S.op("pe", lambda e: e.matmul(p[:, 0:2], lhsT=wt[:, kc, j * 128:(j + 1) * 128], rhs=cT[:, kc, :],
                                                      start=(kc == 0), stop=False), r=[wt, cT], w=[p], inc=False)
                    S.op("pe", lambda e: e.matmul(p[:, 0:2], lhsT=bt[l][0:1, fc * 128:(fc + 1) * 128], rhs=ones2[0:1, :],
                                                  start=False, stop=True), r=[bt[l], ones2], w=[p])
                    S.op("dve", lambda e: e.tensor_copy(out=mo[:, l, fc, :], in_=p[:, 0:2]), r=[p], w=[mo])
                n += 1
        S.dma("sp", out.rearrange("l p c b -> p l c b"), mo[:], r=[mo])
        S.finish([mo], "sp")
    return nc


def emit_norm_tile(S, xt, gm, sh_ap, hT, col0, tp, idb, junk, xn, small, alt):
    S.op("act", lambda e: e.activation(out=junk[:], in_=xt[:], func=AF.Square, accum_out=small[:, 0:1]),
         r=[xt], w=[junk, small])
    S.op("dve", lambda e: e.tensor_scalar(out=small[:, 1:2], in0=small[:, 0:1], scalar1=1.0 / D, scalar2=EPS,
                                          op0=ALU.mult, op1=ALU.add), r=[small], w=[small])
    S.op("act", lambda e: e.activation(out=small[:, 2:3], in_=small[:, 1:2], func=AF.Sqrt), r=[small], w=[small])
    S.op("dve", lambda e: e.reciprocal(out=small[:, 3:4], in_=small[:, 2:3]), r=[small], w=[small])
    S.op("dve", lambda e: e.tensor_scalar(out=xn[:], in0=xt[:], scalar1=small[:, 3:4], scalar2=None, op0=ALU.mult),
         r=[xt, small], w=[xn])
    for g4 in range(4):
        p = tp[g4]
        for c8 in range(8):
            c = g4 * 8 + c8
            S.op("pe", lambda e: e.transpose(out=p[:, c8, :], in_=xn[:, c * 128:(c + 1) * 128], identity=idb[:]),
                 r=[xn, idb], w=[p], inc=(c8 == 7))
        for c8 in range(8):
            c = g4 * 8 + c8
            if (g4 + alt) % 2 == 0:
                S.op("act", lambda e: e.activation(out=hT[c][:, col0:col0 + 128], in_=p[:, c8, :], func=AF.Identity,
                                                   scale=gm[:, c:c + 1], bias=sh_ap[:, c:c + 1]),
                     r=[p, gm], w=[hT[c]])
            else:
                S.op("dve", lambda e: e.tensor_scalar(out=hT[c][:, col0:col0 + 128], in0=p[:, c8, :],
                                                      scalar1=gm[:, c:c + 1], scalar2=sh_ap[:, c:c + 1],
                                                      op0=ALU.mult, op1=ALU.add),
                     r=[p, gm], w=[hT[c]])


def build_LA(TPC=TPC, NMIX=NMIX, CH=1024, dbg=0):
    nc = bass.Bass("TRN2", target_bir_lowering=False)
    x = _din(nc, "x", [TPC, D])
    modfm = _din(nc, "modfm", [128, 192])
    gfm = _din(nc, "gfm", [128, 32])
    win = _din(nc, "win", [D, NMIX])
    identb = _din(nc, "identb", [128, 128], BF16)
    projT = _dout(nc, "projT", [NMIX, TPC])
    hTo = _dout(nc, "hT", [D, TPC], BF16)
    with ExitStack() as st:
        S = Sched(nc, st)
        idb = S.sb("idb", [128, 128], BF16)
        mod = S.sb("mod", [128, 192], F32)
        g = S.sb("g", [128, 32], F32)
        gm = S.sb("gm", [128, 32], F32)
        xb = [S.sb("xb%d" % i, [128, D], F32) for i in range(2)]
        junk = S.sb("junk", [128, D], BF16)
        xn = S.sb("xn", [128, D], BF16)
        small = [S.sb("small%d" % i, [128, 4], F32) for i in range(2)]
        hT = S.sbn("hTs", [128, CH], BF16, 32)
        S.share_dsem(hT)
        wb = [S.sb("wb%d" % i, [128, 32, 512], BF16) for i in range(2)]
        stg = [S.sb("stg%d" % i, [128, 512], F32) for i in range(4)]
        tp = [S.ps("tp%d" % i, [128, 8, 128], BF16) for i in range(4)]
        pa = [S.ps("pa%d" % i, [128, 512]) for i in range(4)]
        S.dma("sp", idb[:], identb, w=[idb])
        S.dma("sp", mod[:], modfm, w=[mod])
        S.dma("sp", g[:], gfm, w=[g])
        S.op("dve", lambda e: e.scalar_tensor_tensor(out=gm[:], in0=mod[:, 32:64], scalar=1.0, in1=g[:],
                                                     op0=ALU.add, op1=ALU.mult), r=[mod, g], w=[gm])
        wv = win.rearrange("(kc p) n -> p kc n", p=128)
        nw = [0]

        def load_w(nb):
            wt = wb[nw[0] % 2]
            nw[0] += 1
            for k0 in range(0, 32, 4):
                S.dma("pool", wt[:, k0:k0 + 4, :], wv[:, k0:k0 + 4, nb * 512:(nb + 1) * 512], w=[wt])
            return wt
        n = 0
        for ch in range(TPC // CH):
            if dbg < 1:
                nxt = load_w(0)
            for tt in range(CH // 128):
                tg = ch * (CH // 128) + tt
                xt = xb[tg % 2]
                S.dma("sp", xt[:], x[tg * 128:(tg + 1) * 128, :], w=[xt])
                emit_norm_tile(S, xt, gm, mod, hT, tt * 128, tp, idb, junk, xn, small[tg % 2], tg)
            if dbg < 2:
                for c in range(32):
                    S.dma("sp", hTo[c * 128:(c + 1) * 128, ch * CH:(ch + 1) * CH], hT[c][:], r=[hT[c]])
            for nb in range(NMIX // 512 if dbg < 1 else 0):
                wt = nxt
                if nb + 1 < NMIX // 512:
                    nxt = load_w(nb + 1)
                for j in range(4):
                    for th in range(CH // 512):
                        p = pa[n % 4]
                        sg = stg[n % 4]
                        for k in range(32):
                            S.op("pe", lambda e: e.matmul(p[:], lhsT=wt[:, k, j * 128:(j + 1) * 128],
                                                          rhs=hT[k][:, th * 512:(th + 1) * 512],
                                                          start=(k == 0), stop=(k == 31)),
                                 r=[wt, hT[k]], w=[p], inc=(k == 31))
                        if n % 2 == 0:
                            S.op("act", lambda e: e.activation(out=sg[:], in_=p[:], func=AF.Copy), r=[p], w=[sg])
                        else:
                            S.op("dve", lambda e: e.tensor_copy(out=sg[:], in_=p[:]), r=[p], w=[sg])
                        row = (nb * 4 + j) * 128
                        S.dma("sp", projT[row:row + 128, ch * CH + th * 512: ch * CH + (th + 1) * 512], sg[:], r=[sg])
                        n += 1
        S.finish(stg + hT, "sp")
    return nc


def _barrier(S):
    deps = {q: S.cnt[q] for q in S.ENG if S.cnt[q] > 0}
    for key in S.dtiles:
        deps[key] = S.dtot[key]
    for q in S.ENG:
        S._wait(q, dict(deps))


GELU_C = 1.5957691216057308
TWO_PI = 2.0 * math.pi
MAGIC = 12582912.0


def build_LB(S_=SEQ, SEGMAX=2048, QCMAX=512, CTMAX=512):
    TS = S_ // 4
    NT = B * S_
    nc = bass.Bass("TRN2", target_bir_lowering=False)
    lx = _din(nc, "lx", [128, NT]); lg = _din(nc, "lg", [128, NT])
    px = _din(nc, "px", [2, 128, NT])
    qd = _din(nc, "q", [128, NT]); kd = _din(nc, "k", [128, NT]); vd = _din(nc, "v", [128, NT])
    cval = _din(nc, "cval", [8, 128, 30 + TS]); cgate = _din(nc, "cgate", [8, 128, 30 + TS])
    ppd = _din(nc, "pp", [128, 40]); cvpd = _din(nc, "cvp", [128, 8, 34])
    wrd = _din(nc, "wr", [128, 128]); wid = _din(nc, "wi", [128, 128]); pwd = _din(nc, "pw", [2, 128, 128])
    identd = _din(nc, "ident", [128, 128]); bdd = _din(nc, "bd", [128, 128]); pmd = _din(nc, "pm", [128, 128])
    onesd = _din(nc, "onesm", [128, 128]); maskd = _din(nc, "masks", [4, 128, 512], BF16)
    posd = _din(nc, "pos", [B, S_], I32); lamvd = _din(nc, "lamv", [128, 256]); sgbd = _din(nc, "sgb", [128, 128])
    linitd = _din(nc, "linit", [128, 2])
    ya = _dout(nc, "ya", [128, NT], BF16); yb = _dout(nc, "yb", [128, NT], BF16)
    yc = _dout(nc, "yc", [128, NT], BF16); yd = _dout(nc, "yd", [8, 128, TS], BF16)
    with ExitStack() as st:
        S = Sched(nc, st)
        P = [S.ps("P%d" % i, [128, 512]) for i in range(8)]
        pp = S.sb("pp_s", [128, 40], F32)
        idt = S.sb("idt", [128, 128], F32)
        S.dma("sp", pp[:], ppd, w=[pp])
        S.dma("sp", idt[:], identd, w=[idt])

        def reg(*tiles):
            pass
        reg(pp, idt)

        with ExitStack() as st2:
            S.stack = st2
            SEG = min(S_, SEGMAX)
            xl = S.sb("a_xl", [128, 3 + SEG], F32); gl = S.sb("a_gl", [128, SEG], F32)
            xa = S.sb("a_xa", [128, SEG], F32); R = S.sb("a_R", [128, SEG], F32)
            I = S.sb("a_I", [128, SEG], F32); A = S.sb("a_A", [128, SEG], F32)
            yo = S.sb("a_yo", [128, SEG], BF16); hc = S.sb("a_hc", [128, 1], F32)
            nsp = S.sb("a_nsp", [128, 2], F32)
            wr = S.sb("a_wr", [128, 128], F32); wi = S.sb("a_wi", [128, 128], F32)
            reg(xl, gl, yo, wr, wi)
            S.dma("sp", wr[:], wrd, w=[wr]); S.dma("sp", wi[:], wid, w=[wi])
            S.op("act", lambda e: e.activation(out=nsp[:, 0:1], in_=pp[:, 7:8], func=AF.Exp, scale=-1.0), r=[pp], w=[nsp])
            S.op("dve", lambda e: e.tensor_scalar_add(out=nsp[:, 0:1], in0=nsp[:, 0:1], scalar1=1.0), r=[nsp], w=[nsp])
            S.op("act", lambda e: e.activation(out=nsp[:, 0:1], in_=nsp[:, 0:1], func=AF.Ln), r=[nsp], w=[nsp])
            S.op("dve", lambda e: e.tensor_scalar(out=nsp[:, 1:2], in0=nsp[:, 0:1], scalar1=-16.0, scalar2=None, op0=ALU.mult), r=[nsp], w=[nsp])
            S.op("dve", lambda e: e.tensor_scalar(out=nsp[:, 0:1], in0=nsp[:, 0:1], scalar1=-8.0, scalar2=None, op0=ALU.mult), r=[nsp], w=[nsp])
            for b in range(B):
                for sg in range(S_ // SEG):
                    t0 = b * S_ + sg * SEG
                    if sg == 0:
                        S.op("dve", lambda e: e.memset(xl[:, 0:3], 0.0), w=[xl])
                    else:
                        S.op("dve", lambda e: e.tensor_copy(out=xl[:, 0:3], in_=xl[:, SEG:SEG + 3]), r=[xl], w=[xl])
                    S.dma("sp", xl[:, 3:3 + SEG], lx[:, t0:t0 + SEG], w=[xl])
                    S.dma("sp", gl[:], lg[:, t0:t0 + SEG], w=[gl])
                    S.op("dve", lambda e: e.tensor_scalar(out=xa[:], in0=xl[:, 0:SEG], scalar1=pp[:, 0:1], scalar2=pp[:, 4:5],
                                                          op0=ALU.mult, op1=ALU.add), r=[xl, pp], w=[xa])
                    for j in range(1, 4):
                        S.op("dve", lambda e: e.scalar_tensor_tensor(out=xa[:], in0=xl[:, j:j + SEG], scalar=pp[:, j:j + 1], in1=xa[:],
                                                                     op0=ALU.mult, op1=ALU.add), r=[xl, xa], w=[xa])
                    for c0 in range(0, SEG, 512):
                        n = min(512, SEG - c0)
                        S.op("pe", lambda e: e.matmul(P[0][:, 0:n], lhsT=wr[:], rhs=xa[:, c0:c0 + n], start=True, stop=True), r=[wr, xa], w=[P[0]])
                        S.op("act", lambda e: e.activation(out=R[:, c0:c0 + n], in_=P[0][:, 0:n], func=AF.Sigmoid, bias=pp[:, 5:6]), r=[P[0]], w=[R])
                        S.op("pe", lambda e: e.matmul(P[1][:, 0:n], lhsT=wi[:], rhs=xa[:, c0:c0 + n], start=True, stop=True), r=[wi, xa], w=[P[1]])
                        S.op("act", lambda e: e.activation(out=I[:, c0:c0 + n], in_=P[1][:, 0:n], func=AF.Sigmoid, bias=pp[:, 6:7]), r=[P[1]], w=[I])
                    S.op("act", lambda e: e.activation(out=A[:], in_=R[:], func=AF.Exp, scale=nsp[:, 0:1]), r=[R, nsp], w=[A])
                    S.op("act", lambda e: e.activation(out=R[:], in_=R[:], func=AF.Exp, scale=nsp[:, 1:2]), r=[R, nsp], w=[R])
                    S.op("dve", lambda e: e.tensor_scalar(out=R[:], in0=R[:], scalar1=-1.0, scalar2=1.0, op0=ALU.mult, op1=ALU.add), r=[R], w=[R])
                    S.op("dve", lambda e: e.tensor_scalar_max(out=R[:], in0=R[:], scalar1=1e-12), r=[R], w=[R])
                    S.op("act", lambda e: e.activation(out=R[:], in_=R[:], func=AF.Sqrt), r=[R], w=[R])
                    S.op("dve", lambda e: e.tensor_tensor(out=I[:], in0=I[:], in1=xa[:], op=ALU.mult), r=[I, xa], w=[I])
                    S.op("dve", lambda e: e.tensor_tensor(out=I[:], in0=I[:], in1=R[:], op=ALU.mult), r=[I, R], w=[I])
                    S.op("dve", lambda e: e.tensor_tensor_scan(out=xa[:], data0=A[:], data1=I[:],
                                                               initial=(0.0 if sg == 0 else hc[:, 0:1]), op0=ALU.mult, op1=ALU.add),
                         r=[A, I, hc], w=[xa])
                    S.op("dve", lambda e: e.tensor_copy(out=hc[:], in_=xa[:, SEG - 1:SEG]), r=[xa], w=[hc])
                    S.op("dve", lambda e: e.tensor_tensor(out=A[:], in0=gl[:], in1=gl[:], op=ALU.mult), r=[gl], w=[A])
                    S.op("dve", lambda e: e.tensor_scalar(out=A[:], in0=A[:], scalar1=0.044715, scalar2=1.0, op0=ALU.mult, op1=ALU.add), r=[A], w=[A])
                    S.op("dve", lambda e: e.tensor_tensor(out=A[:], in0=A[:], in1=gl[:], op=ALU.mult), r=[A, gl], w=[A])
                    S.op("act", lambda e: e.activation(out=A[:], in_=A[:], func=AF.Sigmoid, scale=GELU_C), r=[A], w=[A])
                    S.op("dve", lambda e: e.tensor_tensor(out=A[:], in0=A[:], in1=gl[:], op=ALU.mult), r=[A, gl], w=[A])
                    S.op("dve", lambda e: e.tensor_tensor(out=yo[:], in0=A[:], in1=xa[:], op=ALU.mult), r=[A, xa], w=[yo])
                    S.dma("sp", ya[:, t0:t0 + SEG], yo[:], r=[yo])
            _barrier(S)

        with ExitStack() as st2:
            S.stack = st2
            SEG = min(S_, SEGMAX)
            L = 16 + SEG
            X = [S.sb("b_X%d" % c, [128, L], F32) for c in range(2)]
            Sa = S.sb("b_Sa", [128, L], F32); Sb = S.sb("b_Sb", [128, L], F32)
            acc = S.sb("b_acc", [128, SEG], F32); t16 = S.sb("b_t16", [128, 16], F32)
            dd = [S.sb("b_dd%d" % c, [128, SEG], F32) for c in range(2)]
            yo = S.sb("b_yo", [128, SEG], BF16); pw = S.sb("b_pw", [128, 2, 128], F32); bsc = S.sb("b_bsc", [128, 1], F32)
            reg(X[0], X[1], yo, pw)
            S.dma("sp", pw[:], pwd.rearrange("c p n -> p c n"), w=[pw])
            S.op("dve", lambda e: e.tensor_tensor(out=bsc[:], in0=pp[:, 12:13], in1=pp[:, 13:14], op=ALU.mult), r=[pp], w=[bsc])
            for b in range(B):
                for sg in range(S_ // SEG):
                    t0 = b * S_ + sg * SEG
                    for c in range(2):
                        Xc = X[c]
                        if sg == 0:
                            S.op("dve", lambda e: e.memset(Xc[:, 0:16], 0.0), w=[Xc])
                        else:
                            S.op("dve", lambda e: e.tensor_copy(out=Xc[:, 0:16], in_=Xc[:, SEG:SEG + 16]), r=[Xc], w=[Xc])
                        S.dma("sp", Xc[:, 16:L], px[c][:, t0:t0 + SEG], w=[Xc])
                        S.op("dve", lambda e: e.tensor_tensor(out=Sa[:, 1:L], in0=Xc[:, 1:L], in1=Xc[:, 0:L - 1], op=ALU.add), r=[Xc], w=[Sa])
                        S.op("dve", lambda e: e.tensor_scalar(out=acc[:], in0=Sa[:, 16:L], scalar1=pp[:, 8:9], scalar2=None, op0=ALU.mult), r=[Sa, pp], w=[acc])
                        S.op("dve", lambda e: e.tensor_tensor(out=Sb[:, 3:L], in0=Sa[:, 3:L], in1=Sa[:, 1:L - 2], op=ALU.add), r=[Sa], w=[Sb])
                        S.op("dve", lambda e: e.scalar_tensor_tensor(out=acc[:], in0=Sb[:, 16:L], scalar=pp[:, 9:10], in1=acc[:], op0=ALU.mult, op1=ALU.add), r=[Sb, acc], w=[acc])
                        S.op("dve", lambda e: e.tensor_tensor(out=Sa[:, 7:L], in0=Sb[:, 7:L], in1=Sb[:, 3:L - 4], op=ALU.add), r=[Sb], w=[Sa])
                        S.op("dve", lambda e: e.scalar_tensor_tensor(out=acc[:], in0=Sa[:, 16:L], scalar=pp[:, 10:11], in1=acc[:], op0=ALU.mult, op1=ALU.add), r=[Sa, acc], w=[acc])
                        S.op("dve", lambda e: e.tensor_tensor(out=Sb[:, 15:L], in0=Sa[:, 15:L], in1=Sa[:, 7:L - 8], op=ALU.add), r=[Sa], w=[Sb])
                        S.op("dve", lambda e: e.scalar_tensor_tensor(out=acc[:], in0=Sb[:, 16:L], scalar=pp[:, 11:12], in1=acc[:], op0=ALU.mult, op1=ALU.add), r=[Sb, acc], w=[acc])
                        S.op("dve", lambda e: e.tensor_tensor(out=dd[c][:], in0=acc[:], in1=Xc[:, 16:L], op=ALU.subtract), r=[acc, Xc], w=[dd[c]])
                        if sg == 0:
                            S.op("dve", lambda e: e.tensor_tensor(out=t16[:], in0=acc[:, 0:16], in1=pp[:, 14:30], op=ALU.mult), r=[acc, pp], w=[t16])
                            S.op("dve", lambda e: e.tensor_tensor(out=dd[c][:, 0:16], in0=t16[:], in1=Xc[:, 16:32], op=ALU.subtract), r=[t16, Xc], w=[dd[c]])
                    for c0 in range(0, SEG, 512):
                        n = min(512, SEG - c0)
                        for c in range(2):
                            S.op("pe", lambda e: e.matmul(P[0][:, 0:n], lhsT=pw[:, c, :], rhs=dd[c][:, c0:c0 + n], start=(c == 0), stop=(c == 1)),
                                 r=[pw, dd[c]], w=[P[0]], inc=(c == 1))
                        S.op("act", lambda e: e.activation(out=yo[:, c0:c0 + n], in_=P[0][:, 0:n], func=AF.Identity, scale=pp[:, 13:14], bias=bsc[:, 0:1]),
                             r=[P[0], bsc], w=[yo])
                    S.dma("sp", yb[:, t0:t0 + SEG], yo[:], r=[yo])
            _barrier(S)

        with ExitStack() as st2:
            S.stack = st2
            CT = min(TS, CTMAX)
            LT = 30 + TS
            cvp = S.sb("d_cvp", [128, 8, 34], F32); onesm = S.sb("d_ones", [128, 128], F32)
            Vb = [S.sb("d_V%d" % i, [128, LT], F32) for i in range(2)]
            Gb = [S.sb("d_G%d" % i, [128, LT], F32) for i in range(2)]
            accs = S.sbn("d_acc", [128, TS], F32, 8)
            sq = S.sb("d_sq", [128, CT], F32); M = S.sb("d_M", [128, CT], F32); RS = S.sb("d_RS", [128, CT], F32)
            tt = [S.sb("d_t%d" % i, [128, CT], F32) for i in range(2)]
            yo = [S.sb("d_yo%d" % i, [128, CT], BF16) for i in range(2)]
            reg(cvp, onesm, *Vb, *Gb, *yo)
            S.dma("sp", cvp[:], cvpd, w=[cvp]); S.dma("sp", onesm[:], onesd, w=[onesm])
            idb_d = S.sb("d_idb", [128, 128], BF16)
            S.op("dve", lambda e: e.tensor_copy(out=idb_d[:], in_=idt[:]), r=[idt], w=[idb_d])
            dg = [S.sb("d_dg%d" % i, [128, 31, 128], BF16) for i in range(2)]
            Ub = [S.sb("d_U%d" % i, [128, LT], BF16) for i in range(2)]
            ncv = 0
            for ct in range(8):
                V = Vb[ct % 2]; G = Gb[ct % 2]; U = Ub[ct % 2]; dgt = dg[ct % 2]
                S.dma("sp", V[:], cval[ct], w=[V]); S.dma("sp", G[:], cgate[ct], w=[G])
                S.op("act", lambda e: e.activation(out=G[:], in_=G[:], func=AF.Sigmoid), r=[G], w=[G])
                S.op("dve", lambda e: e.tensor_tensor(out=U[:], in0=V[:], in1=G[:], op=ALU.mult), r=[V, G], w=[U])
                for j in range(31):
                    S.op("pool", lambda e: e.tensor_scalar(out=dgt[:, j, :], in0=idb_d[:], scalar1=cvp[:, ct, j:j + 1], scalar2=None, op0=ALU.mult),
                         r=[idb_d, cvp], w=[dgt])
                ac = accs[ct]
                for c0 in range(0, TS, CT):
                    Pc = P[2 + ncv % 2]
                    ncv += 1
                    for j in range(31):
                        S.op("pe", lambda e: e.matmul(Pc[:, 0:CT], lhsT=dgt[:, j, :], rhs=U[:, c0 + j:c0 + j + CT], start=(j == 0), stop=(j == 30)),
                             r=[dgt, U], w=[Pc], inc=(j == 30))
                    S.op("act", lambda e: e.activation(out=ac[:, c0:c0 + CT], in_=Pc[:, 0:CT], func=AF.Identity, bias=cvp[:, ct, 31:32]), r=[Pc, cvp], w=[ac])
            for c0 in range(0, TS, CT):
                for ct in range(8):
                    S.op("act", lambda e: e.activation(out=sq[:], in_=accs[ct][:, c0:c0 + CT], func=AF.Square), r=[accs[ct]], w=[sq])
                    S.op("pe", lambda e: e.matmul(P[0][:, 0:CT], lhsT=onesm[:], rhs=accs[ct][:, c0:c0 + CT], start=(ct == 0), stop=(ct == 7)),
                         r=[onesm, accs[ct]], w=[P[0]], inc=True)
                    S.op("pe", lambda e: e.matmul(P[1][:, 0:CT], lhsT=onesm[:], rhs=sq[:], start=(ct == 0), stop=(ct == 7)),
                         r=[onesm, sq], w=[P[1]], inc=True)
                S.op("act", lambda e: e.activation(out=M[:], in_=P[0][:, 0:CT], func=AF.Copy), r=[P[0]], w=[M])
                S.op("dve", lambda e: e.tensor_tensor(out=RS[:], in0=M[:], in1=M[:], op=ALU.mult), r=[M], w=[RS])
                S.op("dve", lambda e: e.scalar_tensor_tensor(out=RS[:], in0=RS[:], scalar=-1.0, in1=P[1][:, 0:CT], op0=ALU.mult, op1=ALU.add), r=[RS, P[1]], w=[RS])
                S.op("dve", lambda e: e.tensor_scalar(out=RS[:], in0=RS[:], scalar1=0.0, scalar2=EPS, op0=ALU.max, op1=ALU.add), r=[RS], w=[RS])
                S.op("act", lambda e: e.activation(out=RS[:], in_=RS[:], func=AF.Sqrt), r=[RS], w=[RS])
                S.op("dve", lambda e: e.reciprocal(out=RS[:], in_=RS[:]), r=[RS], w=[RS])
                for ct in range(8):
                    t = tt[ct % 2]; y = yo[ct % 2]
                    S.op("dve", lambda e: e.tensor_tensor(out=t[:], in0=accs[ct][:, c0:c0 + CT], in1=M[:], op=ALU.subtract), r=[accs[ct], M], w=[t])
                    S.op("dve", lambda e: e.tensor_tensor(out=t[:], in0=t[:], in1=RS[:], op=ALU.mult), r=[t, RS], w=[t])
                    S.op("act", lambda e: e.activation(out=y[:], in_=t[:], func=AF.Silu, scale=cvp[:, ct, 32:33], bias=cvp[:, ct, 33:34]), r=[t, cvp], w=[y])
                    S.dma("sp", yd[ct][:, c0:c0 + CT], y[:], r=[y])
            _barrier(S)

        with ExitStack() as st2:
            S.stack = st2
            SEG = min(S_, SEGMAX)
            QC = min(S_, QCMAX)
            NKT = S_ // 128
            bd = S.sb("c_bd", [128, 128], F32); pm = S.sb("c_pm", [128, 128], F32)
            masks = S.sb("c_masks", [128, 4, 512], BF16)
            lamv = S.sb("c_lamv", [128, 256], F32); sgb = S.sb("c_sgb", [128, 128], F32); linit = S.sb("c_linit", [128, 2], F32)
            sm = S.sb("c_sm", [128, 8], F32); ltmp = S.sb("c_ltmp", [128, 64], F32)
            posi = S.sb("c_posi", [128, SEG], I32); ang = S.sb("c_ang", [128, SEG], F32)
            Cc = S.sb("c_C", [128, SEG], F32); Sn = S.sb("c_Sn", [128, SEG], F32)
            kf = S.sb("c_kf", [128, SEG], F32); mm = S.sb("c_mm", [128, SEG], F32)
            X = S.sb("c_X", [128, SEG], F32); SQ = S.sb("c_SQ", [128, SEG], F32); RSa = S.sb("c_RS", [128, SEG], F32)
            qb = S.sb("c_qb", [128, S_], BF16); kb = S.sb("c_kb", [128, S_], BF16)
            Va = S.sb("c_Va", [128, NKT, 129], BF16)
            E = [S.sb("c_E%d" % i, [128, 512], BF16) for i in range(3)]
            on0 = S.sb("c_on0", [128, 4, 128], F32); res = S.sb("c_res", [128, 4, 128], F32)
            rec = S.sb("c_rec", [128, 4], F32); ss = S.sb("c_ss", [128, 8], F32); junk = S.sb("c_junk", [128, 128], F32)
            yt = S.sb("c_yt", [128, 128], F32); yo = [S.sb("c_yo%d" % i, [128, 512], BF16) for i in range(2)]
            reg(bd, pm, masks, lamv, sgb, linit, posi, X, *yo)
            S.dma("sp", bd[:], bdd, w=[bd]); S.dma("sp", pm[:], pmd, w=[pm])
            S.dma("sp", masks[:], maskd.rearrange("m p n -> p m n"), w=[masks])
            S.dma("sp", lamv[:], lamvd, w=[lamv]); S.dma("sp", sgb[:], sgbd, w=[sgb]); S.dma("sp", linit[:], linitd, w=[linit])
            for i2 in range(2):
                S.op("dve", lambda e: e.tensor_tensor(out=ltmp[:], in0=lamv[:, i2 * 128:i2 * 128 + 64], in1=lamv[:, i2 * 128 + 64:i2 * 128 + 128], op=ALU.mult), r=[lamv], w=[ltmp])
                S.op("dve", lambda e: e.reduce_sum(out=sm[:, i2:i2 + 1], in_=ltmp[:], axis=AX.X), r=[ltmp], w=[sm])
            S.op("act", lambda e: e.activation(out=sm[:, 0:2], in_=sm[:, 0:2], func=AF.Exp), r=[sm], w=[sm])
            S.op("dve", lambda e: e.tensor_tensor(out=sm[:, 2:3], in0=sm[:, 1:2], in1=sm[:, 0:1], op=ALU.subtract), r=[sm], w=[sm])
            S.op("dve", lambda e: e.tensor_tensor(out=sm[:, 2:3], in0=sm[:, 2:3], in1=linit[:, 0:1], op=ALU.subtract), r=[sm, linit], w=[sm])
            S.op("dve", lambda e: e.tensor_scalar(out=sgb[:], in0=sgb[:], scalar1=linit[:, 1:2], scalar2=None, op0=ALU.mult), r=[sgb, linit], w=[sgb])
            S.op("dve", lambda e: e.memset(Va[:, :, 128:129], 1.0), w=[Va])
            for b in range(B):
                for sg in range(S_ // SEG):
                    s0 = sg * SEG
                    t0 = b * S_ + s0
                    S.dma("sp", posi[:], posd[b:b + 1, s0:s0 + SEG].partition_broadcast(128), w=[posi])
                    S.op("dve", lambda e: e.tensor_copy(out=ang[:], in_=posi[:]), r=[posi], w=[ang])
                    S.op("dve", lambda e: e.tensor_scalar(out=ang[:], in0=ang[:], scalar1=pp[:, 32:33], scalar2=None, op0=ALU.mult), r=[ang, pp], w=[ang])
                    for which in range(2):
                        dst = Sn if which == 0 else Cc
                        src = ang
                        if which == 1:
                            S.op("dve", lambda e: e.tensor_scalar_add(out=mm[:], in0=ang[:], scalar1=math.pi / 2), r=[ang], w=[mm])
                            src = mm
                        S.op("dve", lambda e: e.tensor_scalar(out=kf[:], in0=src[:], scalar1=1.0 / TWO_PI, scalar2=MAGIC, op0=ALU.mult, op1=ALU.add), r=[src], w=[kf])
                        S.op("dve", lambda e: e.tensor_scalar_add(out=kf[:], in0=kf[:], scalar1=-MAGIC), r=[kf], w=[kf])
                        S.op("dve", lambda e: e.scalar_tensor_tensor(out=mm[:], in0=kf[:], scalar=-TWO_PI, in1=src[:], op0=ALU.mult, op1=ALU.add), r=[kf, src], w=[mm])
                        S.op("dve", lambda e: e.tensor_scalar(out=mm[:], in0=mm[:], scalar1=math.pi, scalar2=-math.pi, op0=ALU.min, op1=ALU.max), r=[mm], w=[mm])
                        if which == 0:
                            S.op("act", lambda e: e.activation(out=dst[:], in_=mm[:], func=AF.Sin, scale=pp[:, 33:34]), r=[mm, pp], w=[dst])
                        else:
                            S.op("act", lambda e: e.activation(out=dst[:], in_=mm[:], func=AF.Sin), r=[mm], w=[dst])
                    for which, (src_d, dstb, gcol, scl) in enumerate(((qd, qb, 30, 0.125), (kd, kb, 31, 1.0))):
                        S.dma("sp", X[:], src_d[:, t0:t0 + SEG], w=[X])
                        S.op("act", lambda e: e.activation(out=SQ[:], in_=X[:], func=AF.Square), r=[X], w=[SQ])
                        for c0 in range(0, SEG, 512):
                            n = min(512, SEG - c0)
                            S.op("pe", lambda e: e.matmul(P[0][:, 0:n], lhsT=bd[:], rhs=SQ[:, c0:c0 + n], start=True, stop=True), r=[bd, SQ], w=[P[0]])
                            S.op("dve", lambda e: e.tensor_scalar_add(out=RSa[:, c0:c0 + n], in0=P[0][:, 0:n], scalar1=EPS), r=[P[0]], w=[RSa])
                        S.op("act", lambda e: e.activation(out=RSa[:], in_=RSa[:], func=AF.Sqrt), r=[RSa], w=[RSa])
                        S.op("dve", lambda e: e.reciprocal(out=RSa[:], in_=RSa[:]), r=[RSa], w=[RSa])
                        S.op("dve", lambda e: e.scalar_tensor_tensor(out=X[:], in0=X[:], scalar=pp[:, gcol:gcol + 1], in1=RSa[:], op0=ALU.mult, op1=ALU.mult), r=[X, RSa, pp], w=[X])
                        for c0 in range(0, SEG, 512):
                            n = min(512, SEG - c0)
                            S.op("pe", lambda e: e.matmul(P[1][:, 0:n], lhsT=pm[:], rhs=X[:, c0:c0 + n], start=True, stop=True), r=[pm, X], w=[P[1]])
                            S.op("dve", lambda e: e.tensor_tensor(out=SQ[:, c0:c0 + n], in0=P[1][:, 0:n], in1=Sn[:, c0:c0 + n], op=ALU.mult), r=[P[1], Sn], w=[SQ])
                        S.op("dve", lambda e: e.tensor_tensor(out=X[:], in0=X[:], in1=Cc[:], op=ALU.mult), r=[X, Cc], w=[X])
                        S.op("dve", lambda e: e.tensor_tensor(out=X[:], in0=X[:], in1=SQ[:], op=ALU.add), r=[X, SQ], w=[X])
                        S.op("act", lambda e: e.activation(out=dstb[:, s0:s0 + SEG], in_=X[:], func=AF.Copy, scale=scl), r=[X], w=[dstb])
                    S.dma("sp", X[:], vd[:, t0:t0 + SEG], w=[X])
                    for g0 in range(0, SEG // 128, 4):
                        nj = min(4, SEG // 128 - g0)
                        for j in range(nj):
                            kt = g0 + j
                            S.op("pe", lambda e: e.transpose(out=P[2][:, j * 128:(j + 1) * 128], in_=X[:, kt * 128:(kt + 1) * 128], identity=idt[:]),
                                 r=[X, idt], w=[P[2]], inc=(j == nj - 1))
                        k0 = s0 // 128 + g0
                        S.op("act", lambda e: e.activation(out=Va[:, k0:k0 + nj, 0:128], in_=P[2][:, 0:nj * 128].rearrange("p (a b) -> p a b", a=nj), func=AF.Copy), r=[P[2]], w=[Va])
                NQT = QC // 128
                steps = []
                for qc in range(S_ // QC):
                    for c in range(2):
                        for kt in range((qc + 1) * NQT):
                            steps.append((qc, c, kt))

                def emit_st(i):
                    qc_, c_, kt_ = steps[i]
                    lo_, hi_ = c_ * 64, (c_ + 1) * 64
                    Ps_ = P[i % 3]
                    S.op("pe", lambda e: e.matmul(Ps_[:, 0:QC], lhsT=kb[lo_:hi_, kt_ * 128:(kt_ + 1) * 128], rhs=qb[lo_:hi_, qc_ * QC:(qc_ + 1) * QC],
                                                  start=True, stop=True), r=[kb, qb], w=[Ps_])
                emit_st(0)
                for i, (qc, c, kt) in enumerate(steps):
                    Ps = P[i % 3]
                    Et = E[i % 3]
                    S.op("act", lambda e: e.activation(out=Et[:, 0:QC], in_=Ps[:, 0:QC], func=AF.Exp), r=[Ps], w=[Et])
                    if i + 1 < len(steps):
                        emit_st(i + 1)
                    m = kt - qc * NQT
                    if m >= 0:
                        S.op("dve", lambda e: e.tensor_tensor(out=Et[:, m * 128:(m + 1) * 128], in0=Et[:, m * 128:(m + 1) * 128],
                                                              in1=masks[:, 0, 0:128], op=ALU.mult), r=[Et, masks], w=[Et])
                    for qt in range(NQT):
                        if m > qt:
                            continue
                        last = (kt == qc * NQT + qt)
                        S.op("pe", lambda e: e.matmul(P[4 + qt][:, 0:129], lhsT=Et[:, qt * 128:(qt + 1) * 128], rhs=Va[:, kt, :],
                                                      start=(kt == 0), stop=last), r=[Et, Va], w=[P[4 + qt]], inc=True)
                    if kt != (qc + 1) * NQT - 1:
                        continue
                    for qt in range(NQT):
                        Po = P[4 + qt]
                        S.op("dve", lambda e: e.reciprocal(out=rec[:, qt:qt + 1], in_=Po[:, 128:129]), r=[Po], w=[rec])
                        if c == 0:
                            S.op("dve", lambda e: e.tensor_scalar(out=on0[:, qt, :], in0=Po[:, 0:128], scalar1=rec[:, qt:qt + 1], scalar2=None, op0=ALU.mult), r=[Po, rec], w=[on0])
                        else:
                            S.op("dve", lambda e: e.tensor_scalar(out=res[:, qt, :], in0=Po[:, 0:128], scalar1=rec[:, qt:qt + 1], scalar2=None, op0=ALU.mult), r=[Po, rec], w=[res])
                            S.op("dve", lambda e: e.scalar_tensor_tensor(out=res[:, qt, :], in0=res[:, qt, :], scalar=sm[:, 2:3], in1=on0[:, qt, :], op0=ALU.mult, op1=ALU.add), r=[res, on0, sm], w=[res])
                    if c == 0:
                        continue
                    y = yo[qc % 2]
                    for qt in range(QC // 128):
                        S.op("act", lambda e: e.activation(out=junk[:], in_=res[:, qt, :], func=AF.Square, accum_out=ss[:, qt:qt + 1]), r=[res], w=[junk, ss])
                    S.op("dve", lambda e: e.tensor_scalar(out=ss[:, 4:8], in0=ss[:, 0:4], scalar1=1.0 / 128, scalar2=EPS, op0=ALU.mult, op1=ALU.add), r=[ss], w=[ss])
                    S.op("act", lambda e: e.activation(out=ss[:, 4:8], in_=ss[:, 4:8], func=AF.Sqrt), r=[ss], w=[ss])
                    S.op("dve", lambda e: e.reciprocal(out=ss[:, 4:8], in_=ss[:, 4:8]), r=[ss], w=[ss])
                    for qt in range(QC // 128):
                        S.op("dve", lambda e: e.scalar_tensor_tensor(out=yt[:], in0=res[:, qt, :], scalar=ss[:, 4 + qt:5 + qt], in1=sgb[:], op0=ALU.mult, op1=ALU.mult), r=[res, ss, sgb], w=[yt])
                        S.op("pe", lambda e: e.transpose(out=P[3][:, qt * 128:(qt + 1) * 128], in_=yt[:], identity=idt[:]), r=[yt, idt], w=[P[3]], inc=True)
                    S.op("act", lambda e: e.activation(out=y[:, 0:QC], in_=P[3][:, 0:QC], func=AF.Copy), r=[P[3]], w=[y])
                    S.dma("sp", yc[:, b * S_ + qc * QC: b * S_ + (qc + 1) * QC], y[:, 0:QC], r=[y])
            _barrier(S)
        S.stack = st
        S.finish([pp], "sp")
    return nc


def build_LC(TPC=TPC):
    CH = min(TPC, 512)
    NTT = CH // 128
    nc = bass.Bass("TRN2", target_bir_lowering=False)
    yTd = _din(nc, "yT", [D, TPC], BF16); hTd = _din(nc, "hT", [D, TPC], BF16)
    xd = _din(nc, "x", [TPC, D])
    modfm = _din(nc, "modfm", [128, 192]); g2fm = _din(nc, "g2fm", [128, 32]); bgfm = _din(nc, "bgfm", [128, 4, 32])
    wg = _din(nc, "wg", [D, 4 * D]); wbr = _din(nc, "wbr", [4, DB, D]); wo = _din(nc, "wo", [D, D])
    wrt = _din(nc, "wrt", [D, 36]); rb = _din(nc, "rb", [1, 36]); identd = _din(nc, "ident", [128, 128])
    xmid = _dout(nc, "xmid", [TPC, D]); h2To = _dout(nc, "h2T", [D, TPC], BF16); combo = _dout(nc, "comb", [TPC, 32])
    with ExitStack() as st:
        S = Sched(nc, st)
        P = [S.ps("P%d" % i, [128, 512]) for i in range(8)]
        mod = S.sb("mod", [128, 192], F32); g2 = S.sb("g2", [128, 32], F32); gm2 = S.sb("gm2", [128, 32], F32)
        bg = S.sb("bg", [128, 4, 32], F32); idt = S.sb("idt", [128, 128], F32)
        wr_s = S.sb("wr_s", [128, 32, 36], F32); rb_s = S.sb("rb_s", [1, 36], F32); ones1 = S.sb("ones1", [1, 128], F32)
        mg = S.sbn("mg", [128, CH], BF16, 32)
        wb = [S.sb("wb%d" % i, [128, 32, 256], BF16) for i in range(2)]
        S.dma("sp", mod[:], modfm, w=[mod]); S.dma("sp", g2[:], g2fm, w=[g2]); S.dma("sp", bg[:], bgfm, w=[bg])
        S.dma("sp", idt[:], identd, w=[idt]); S.dma("sp", rb_s[:], rb, w=[rb_s])
        S.dma("sp", wr_s[:], wrt.rearrange("(c p) n -> p c n", p=128), w=[wr_s])
        S.op("dve", lambda e: e.memset(ones1[:], 1.0), w=[ones1])
        S.op("dve", lambda e: e.scalar_tensor_tensor(out=gm2[:], in0=mod[:, 128:160], scalar=1.0, in1=g2[:], op0=ALU.add, op1=ALU.mult), r=[mod, g2], w=[gm2])
        wgv = wg.rearrange("(kc p) n -> p kc n", p=128)
        wov = wo.rearrange("(kc p) n -> p kc n", p=128)
        nl = [0]

        def load_big(src, col0):
            wt = wb[nl[0] % 2]
            nl[0] += 1
            for k0 in range(0, 32, 8):
                S.dma("pool", wt[:, k0:k0 + 8, :], src[:, k0:k0 + 8, col0:col0 + 256], w=[wt])
            return wt
        nbb = [0]
        for ch in range(TPC // CH):
            c0 = ch * CH
            with ExitStack() as st2:
                S.stack = st2
                yTs = S.sbn("yTs%d" % ch, [128, CH], BF16, 32)
                hTs = S.sbn("hTs%d" % ch, [128, CH], BF16, 32)
                S.share_dsem(yTs + hTs)
                wbb = [S.sb("wbb%d_%d" % (ch, i), [128, 8, 256], BF16) for i in range(2)]
                acc = [S.sb("acc%d_%d" % (ch, i), [128, CH], F32) for i in range(2)]
                gate = [S.sb("gate%d_%d" % (ch, i), [128, CH], F32) for i in range(2)]
                tmp = [S.sb("tmp%d_%d" % (ch, i), [128, CH], F32) for i in range(2)]
                for c in range(32):
                    S.dma("sp", yTs[c][:], yTd[c * 128:(c + 1) * 128, c0:c0 + CH], w=[yTs[c]])
                    S.dma("sp", hTs[c][:], hTd[c * 128:(c + 1) * 128, c0:c0 + CH], w=[hTs[c]])
                nev = 0
                for nb2 in range(16):
                    for b in range(4):
                        wt = load_big(wgv, b * D + nb2 * 256)
                        wbt = wbb[nbb[0] % 2]
                        nbb[0] += 1
                        S.dma("pool", wbt[:], wbr[b].rearrange("(c p) n -> p c n", p=128)[:, :, nb2 * 256:(nb2 + 1) * 256], w=[wbt])
                        for j in range(2):
                            n = nb2 * 2 + j
                            Pg = P[(nev % 2) * 2]; Pz = P[(nev % 2) * 2 + 1]
                            gt = gate[nev % 2]; tp_ = tmp[nev % 2]
                            nev += 1
                            for k in range(32):
                                S.op("pe", lambda e: e.matmul(Pg[:, 0:CH], lhsT=wt[:, k, j * 128:(j + 1) * 128], rhs=hTs[k][:], start=(k == 0), stop=(k == 31)),
                                     r=[wt, hTs[k]], w=[Pg], inc=(k == 31))
                            S.op("act", lambda e: e.activation(out=gt[:], in_=Pg[:, 0:CH], func=AF.Sigmoid, bias=bg[:, b, n:n + 1]), r=[Pg, bg], w=[gt])
                            for k in range(8):
                                S.op("pe", lambda e: e.matmul(Pz[:, 0:CH], lhsT=wbt[:, k, j * 128:(j + 1) * 128], rhs=yTs[b * 8 + k][:], start=(k == 0), stop=(k == 7)),
                                     r=[wbt, yTs[b * 8 + k]], w=[Pz], inc=(k == 7))
                            a = acc[j]
                            if b == 0:
                                S.op("dve", lambda e: e.tensor_tensor(out=a[:], in0=Pz[:, 0:CH], in1=gt[:], op=ALU.mult), r=[Pz, gt], w=[a])
                            else:
                                S.op("dve", lambda e: e.tensor_tensor(out=tp_[:], in0=Pz[:, 0:CH], in1=gt[:], op=ALU.mult), r=[Pz, gt], w=[tp_])
                                if b < 3:
                                    S.op("dve", lambda e: e.tensor_tensor(out=a[:], in0=a[:], in1=tp_[:], op=ALU.add), r=[a, tp_], w=[a])
                                else:
                                    S.op("dve", lambda e: e.tensor_tensor(out=mg[n][:], in0=a[:], in1=tp_[:], op=ALU.add), r=[a, tp_], w=[mg[n]])
                _barrier(S)
            with ExitStack() as st2:
                S.stack = st2
                xtok = S.sb("xtok%d" % ch, [128, NTT, D], F32)
                h2s = S.sbn("h2s%d" % ch, [128, CH], BF16, 32)
                S.share_dsem(h2s)
                dT = [S.sb("dT%d_%d" % (ch, i), [128, CH], F32) for i in range(2)]
                xn = S.sb("xn%d" % ch, [128, D], F32)
                h2f = [S.sb("h2f%d_%d" % (ch, i), [128, 4, 128], F32) for i in range(2)]
                sm = S.sb("sm%d" % ch, [128, 16], F32); lg = S.sb("lg%d" % ch, [128, 36], F32)
                em = S.sb("em%d" % ch, [128, 32], F32); t8 = S.sb("t8%d" % ch, [128, 8], F32); oh = S.sb("oh%d" % ch, [128, 4], F32)
                cb1 = S.sb("cb1%d" % ch, [128, 32], F32); cb2 = S.sb("cb2%d" % ch, [128, 32], F32)
                for tt in range(NTT):
                    S.dma("sp", xtok[:, tt, :], xd[c0 + tt * 128:c0 + (tt + 1) * 128, :], w=[xtok])
                nev = 0
                for nb2 in range(16):
                    wt = load_big(wov, nb2 * 256)
                    for j in range(2):
                        n = nb2 * 2 + j
                        Pd = P[nev % 2]; Pt = P[2 + nev % 2]; dt_ = dT[nev % 2]
                        nev += 1
                        for k in range(32):
                            S.op("pe", lambda e: e.matmul(Pd[:, 0:CH], lhsT=wt[:, k, j * 128:(j + 1) * 128], rhs=mg[k][:], start=(k == 0), stop=(k == 31)),
                                 r=[wt, mg[k]], w=[Pd], inc=(k == 31))
                        S.op("act", lambda e: e.activation(out=dt_[:], in_=Pd[:, 0:CH], func=AF.Identity, scale=mod[:, 64 + n:65 + n]), r=[Pd, mod], w=[dt_])
                        for tt in range(NTT):
                            S.op("pe", lambda e: e.transpose(out=Pt[:, tt * 128:(tt + 1) * 128], in_=dt_[:, tt * 128:(tt + 1) * 128], identity=idt[:]),
                                 r=[dt_, idt], w=[Pt], inc=(tt == NTT - 1))
                        S.op("dve", lambda e: e.tensor_tensor(out=xtok[:, :, n * 128:(n + 1) * 128], in0=xtok[:, :, n * 128:(n + 1) * 128],
                                                              in1=Pt[:, 0:CH].rearrange("p (a b) -> p a b", a=NTT), op=ALU.add), r=[xtok, Pt], w=[xtok])
                for tt in range(NTT):
                    S.dma("sp", xmid[c0 + tt * 128:c0 + (tt + 1) * 128, :], xtok[:, tt, :], r=[xtok])
                    S.op("act", lambda e: e.activation(out=xn[:], in_=xtok[:, tt, :], func=AF.Square, accum_out=sm[:, 0:1]), r=[xtok], w=[xn, sm])
                    S.op("dve", lambda e: e.tensor_scalar(out=sm[:, 1:2], in0=sm[:, 0:1], scalar1=1.0 / D, scalar2=EPS, op0=ALU.mult, op1=ALU.add), r=[sm], w=[sm])
                    S.op("act", lambda e: e.activation(out=sm[:, 2:3], in_=sm[:, 1:2], func=AF.Sqrt), r=[sm], w=[sm])
                    S.op("dve", lambda e: e.reciprocal(out=sm[:, 3:4], in_=sm[:, 2:3]), r=[sm], w=[sm])
                    S.op("dve", lambda e: e.tensor_scalar(out=xn[:], in0=xtok[:, tt, :], scalar1=sm[:, 3:4], scalar2=None, op0=ALU.mult), r=[xtok, sm], w=[xn])
                    Pl = P[7]
                    for g4 in range(8):
                        Pt = P[4 + g4 % 2]; hf = h2f[g4 % 2]
                        for c4 in range(4):
                            c = g4 * 4 + c4
                            S.op("pe", lambda e: e.transpose(out=Pt[:, c4 * 128:(c4 + 1) * 128], in_=xn[:, c * 128:(c + 1) * 128], identity=idt[:]),
                                 r=[xn, idt], w=[Pt], inc=(c4 == 3))
                        for c4 in range(4):
                            c = g4 * 4 + c4
                            if g4 % 2 == 0:
                                S.op("act", lambda e: e.activation(out=hf[:, c4, :], in_=Pt[:, c4 * 128:(c4 + 1) * 128], func=AF.Identity,
                                                                   scale=gm2[:, c:c + 1], bias=mod[:, 96 + c:97 + c]), r=[Pt, gm2, mod], w=[hf])
                            else:
                                S.op("dve", lambda e: e.tensor_scalar(out=hf[:, c4, :], in0=Pt[:, c4 * 128:(c4 + 1) * 128], scalar1=gm2[:, c:c + 1],
                                                                      scalar2=mod[:, 96 + c:97 + c], op0=ALU.mult, op1=ALU.add), r=[Pt, gm2, mod], w=[hf])
                        for c4 in range(4):
                            c = g4 * 4 + c4
                            S.op("pool", lambda e: e.tensor_copy(out=h2s[c][:, tt * 128:(tt + 1) * 128], in_=hf[:, c4, :]), r=[hf], w=[h2s[c]])
                            S.op("pe", lambda e: e.matmul(Pl[:, 0:36], lhsT=hf[:, c4, :], rhs=wr_s[:, c, :], start=(c == 0), stop=False),
                                 r=[hf, wr_s], w=[Pl], inc=(c4 == 3))
                    S.op("pe", lambda e: e.matmul(Pl[:, 0:36], lhsT=ones1[0:1, :], rhs=rb_s[0:1, :], start=False, stop=True), r=[ones1, rb_s], w=[Pl])
                    S.op("dve", lambda e: e.tensor_copy(out=lg[:], in_=Pl[:, 0:36]), r=[Pl], w=[lg])
                    S.op("dve", lambda e: e.reduce_max(out=sm[:, 4:5], in_=lg[:, 0:4], axis=AX.X), r=[lg], w=[sm])
                    S.op("dve", lambda e: e.tensor_scalar(out=sm[:, 5:6], in0=sm[:, 4:5], scalar1=-1.0, scalar2=None, op0=ALU.mult), r=[sm], w=[sm])
                    S.op("act", lambda e: e.activation(out=oh[:], in_=lg[:, 0:4], func=AF.Exp, bias=sm[:, 5:6], accum_out=sm[:, 6:7]), r=[lg, sm], w=[oh, sm])
                    S.op("dve", lambda e: e.reciprocal(out=sm[:, 7:8], in_=sm[:, 6:7]), r=[sm], w=[sm])
                    S.op("dve", lambda e: e.tensor_scalar(out=oh[:], in0=lg[:, 0:4], scalar1=sm[:, 4:5], scalar2=None, op0=ALU.is_equal), r=[lg, sm], w=[oh])
                    S.op("dve", lambda e: e.tensor_scalar(out=oh[:], in0=oh[:], scalar1=-1.0, scalar2=1e30, op0=ALU.add, op1=ALU.mult), r=[oh], w=[oh])
                    for g in range(4):
                        S.op("dve", lambda e: e.tensor_scalar(out=em[:, g * 8:(g + 1) * 8], in0=lg[:, 4 + g * 8:12 + g * 8], scalar1=oh[:, g:g + 1], scalar2=None, op0=ALU.add), r=[lg, oh], w=[em])
                    S.op("dve", lambda e: e.max(out=t8[:], in_=em[:]), r=[em], w=[t8])
                    S.op("dve", lambda e: e.tensor_tensor(out=sm[:, 8:9], in0=t8[:, 1:2], in1=t8[:, 0:1], op=ALU.subtract), r=[t8], w=[sm])
                    S.op("act", lambda e: e.activation(out=sm[:, 8:9], in_=sm[:, 8:9], func=AF.Exp), r=[sm], w=[sm])
                    S.op("dve", lambda e: e.tensor_scalar_add(out=sm[:, 8:9], in0=sm[:, 8:9], scalar1=1.0), r=[sm], w=[sm])
                    S.op("dve", lambda e: e.reciprocal(out=sm[:, 9:10], in_=sm[:, 8:9]), r=[sm], w=[sm])
                    S.op("dve", lambda e: e.tensor_tensor(out=sm[:, 10:11], in0=sm[:, 9:10], in1=sm[:, 7:8], op=ALU.mult), r=[sm], w=[sm])
                    S.op("dve", lambda e: e.tensor_tensor(out=sm[:, 11:12], in0=sm[:, 7:8], in1=sm[:, 10:11], op=ALU.subtract), r=[sm], w=[sm])
                    S.op("dve", lambda e: e.tensor_scalar(out=cb1[:], in0=em[:], scalar1=t8[:, 0:1], scalar2=sm[:, 10:11], op0=ALU.is_equal, op1=ALU.mult), r=[em, t8, sm], w=[cb1])
                    S.op("dve", lambda e: e.tensor_scalar(out=cb2[:], in0=em[:], scalar1=t8[:, 1:2], scalar2=sm[:, 11:12], op0=ALU.is_equal, op1=ALU.mult), r=[em, t8, sm], w=[cb2])
                    S.op("dve", lambda e: e.tensor_tensor(out=cb1[:], in0=cb1[:], in1=cb2[:], op=ALU.add), r=[cb1, cb2], w=[cb1])
                    S.dma("sp", combo[c0 + tt * 128:c0 + (tt + 1) * 128, :], cb1[:], r=[cb1])
                for c in range(32):
                    S.dma("sp", h2To[c * 128:(c + 1) * 128, c0:c0 + CH], h2s[c][:], r=[h2s[c]])
                _barrier(S)
        S.stack = st
        S.finish([mod], "sp")
    return nc


def build_LD(TPC=TPC):
    CH = min(TPC, 512)
    NTT = CH // 128
    nc = bass.Bass("TRN2", target_bir_lowering=False)
    h2Td = _din(nc, "h2T", [D, TPC], BF16); combd = _din(nc, "combT", [32, TPC]); xmd = _din(nc, "xmid", [TPC, D])
    g2bd = _din(nc, "g2b", [128, D]); w1 = _din(nc, "w1", [32, D, 512]); w3 = _din(nc, "w3", [32, D, 512]); w2 = _din(nc, "w2", [32, 512, D])
    xo = _dout(nc, "xout", [TPC, D])
    with ExitStack() as st:
        S = Sched(nc, st)
        P = [S.ps("P%d" % i, [128, 512]) for i in range(8)]
        g2b = S.sb("g2b_s", [128, D], F32)
        h2s = S.sbn("h2s", [128, CH], BF16, 32)
        S.share_dsem(h2s)
        hid = S.sbn("hid", [128, CH], BF16, 16)
        xacc = S.sb("xacc", [128, NTT, D], F32)
        wu = [S.sb("wu%d" % i, [128, 32, 128], BF16) for i in range(4)]
        w2u = [S.sb("w2u%d" % i, [128, 16, 256], BF16) for i in range(2)]
        cbb = [S.sb("cbb%d" % i, [128, CH], F32) for i in range(2)]
        sl = [S.sb("sl%d" % i, [128, CH], F32) for i in range(2)]
        tm = [S.sb("tm%d" % i, [128, CH], F32) for i in range(2)]
        t2 = [S.sb("t2_%d" % i, [128, 256], F32) for i in range(2)]
        for q4 in range(4):
            S.dma("sp", g2b[:, q4 * 1024:(q4 + 1) * 1024], g2bd[:, q4 * 1024:(q4 + 1) * 1024], w=[g2b])
        nu = [0]

        def load_u(src_e, col0):
            wt = wu[nu[0] % 4]
            nu[0] += 1
            v = src_e.rearrange("(kc p) n -> p kc n", p=128)
            for k0 in range(0, 32, 8):
                S.dma("pool", wt[:, k0:k0 + 8, :], v[:, k0:k0 + 8, col0:col0 + 128], w=[wt])
            return wt
        n2 = [0]
        for ch in range(TPC // CH):
            c0 = ch * CH
            for c in range(32):
                S.dma("sp", h2s[c][:], h2Td[c * 128:(c + 1) * 128, c0:c0 + CH], w=[h2s[c]])
            for tt in range(NTT):
                S.dma("sp", xacc[:, tt, :], xmd[c0 + tt * 128:c0 + (tt + 1) * 128, :], w=[xacc])
            nev = 0
            for hg in range(8):
                for ei in range(4):
                    ex = hg * 4 + ei
                    cb = cbb[ex % 2]
                    S.dma("sp", cb[:], combd[ex:ex + 1, c0:c0 + CH].partition_broadcast(128), w=[cb])
                    for j in range(4):
                        u1 = load_u(w1[ex], j * 128)
                        u3 = load_u(w3[ex], j * 128)
                        for jj in range(1):
                            Pa = P[(nev % 2) * 2]; Pb = P[(nev % 2) * 2 + 1]
                            s_ = sl[nev % 2]; t_ = tm[nev % 2]
                            nev += 1
                            for k in range(32):
                                S.op("pe", lambda e: e.matmul(Pa[:, 0:CH], lhsT=u1[:, k, :], rhs=h2s[k][:], start=(k == 0), stop=(k == 31)),
                                     r=[u1, h2s[k]], w=[Pa], inc=(k == 31))
                            for k in range(32):
                                S.op("pe", lambda e: e.matmul(Pb[:, 0:CH], lhsT=u3[:, k, :], rhs=h2s[k][:], start=(k == 0), stop=(k == 31)),
                                     r=[u3, h2s[k]], w=[Pb], inc=(k == 31))
                            S.op("act", lambda e: e.activation(out=s_[:], in_=Pa[:, 0:CH], func=AF.Silu), r=[Pa], w=[s_])
                            S.op("dve", lambda e: e.tensor_tensor(out=t_[:], in0=Pb[:, 0:CH], in1=s_[:], op=ALU.mult), r=[Pb, s_], w=[t_])
                            S.op("dve", lambda e: e.tensor_tensor(out=hid[ei * 4 + j][:], in0=t_[:], in1=cb[:], op=ALU.mult), r=[t_, cb], w=[hid[ei * 4 + j]])
                w2v = w2[hg * 4:(hg + 1) * 4].rearrange("e (j p) n -> p (e j) n", p=128)
                for nb in range(16):
                    wt = w2u[n2[0] % 2]
                    n2[0] += 1
                    for k0 in range(0, 16, 8):
                        S.dma("pool", wt[:, k0:k0 + 8, :], w2v[:, k0:k0 + 8, nb * 256:(nb + 1) * 256], w=[wt])
                    for tt in range(NTT):
                        Po = P[4 + (nb * NTT + tt) % 2]
                        tq = t2[(nb * NTT + tt) % 2]
                        for m in range(16):
                            S.op("pe", lambda e: e.matmul(Po[:, 0:256], lhsT=hid[m][:, tt * 128:(tt + 1) * 128], rhs=wt[:, m, :], start=(m == 0), stop=(m == 15)),
                                 r=[hid[m], wt], w=[Po], inc=(m == 15))
                        S.op("dve", lambda e: e.tensor_tensor(out=tq[:], in0=Po[:, 0:256], in1=g2b[:, nb * 256:(nb + 1) * 256], op=ALU.mult), r=[Po, g2b], w=[tq])
                        S.op("dve", lambda e: e.tensor_tensor(out=xacc[:, tt, nb * 256:(nb + 1) * 256], in0=xacc[:, tt, nb * 256:(nb + 1) * 256], in1=tq[:], op=ALU.add), r=[xacc, tq], w=[xacc])
            for tt in range(NTT):
                S.dma("sp", xo[c0 + tt * 128:c0 + (tt + 1) * 128, :], xacc[:, tt, :], r=[xacc])
        S.finish([xacc], "sp")
    return nc


BF = ml_dtypes.bfloat16
POOLW = (2, 4, 8, 16)


def _lb_consts():
    ident = np.eye(128, dtype=np.float32)
    bd = np.zeros((128, 128), np.float32)
    bd[:64, :64] = 1.0 / 64
    bd[64:, 64:] = 1.0 / 64
    pm = np.zeros((128, 128), np.float32)
    invf = np.zeros(128, np.float32)
    sgn = np.zeros(128, np.float32)
    inv_freq = (np.float32(500000.0) ** (-np.arange(0, 16, 2, dtype=np.float32) / np.float32(16))).astype(np.float32)
    for p in range(128):
        d = p % 64
        if d < 8:
            pm[p + 8, p] = 1
            invf[p] = inv_freq[d]
            sgn[p] = -1
        elif d < 16:
            pm[p - 8, p] = 1
            invf[p] = inv_freq[d - 8]
            sgn[p] = 1
    onesm = np.full((128, 128), 1.0 / 1024, np.float32)
    masks = np.zeros((4, 128, 512), np.float32)
    pp_, ff = np.meshgrid(np.arange(128), np.arange(512), indexing='ij')
    for m in range(4):
        masks[m] = (ff - pp_ - 128 * m >= 0)
    return dict(ident=ident, bd=bd, pm=pm, onesm=onesm, masks=masks.astype(BF)), invf, sgn


def _lb_maps(projT, inp, l, S_):
    consts, invf, sgn = _lb_consts()
    TS = S_ // 4
    lambda_init = 0.8 - 0.6 * math.exp(-0.3 * l)
    maps = []
    for i in range(8):
        g, half = i // 2, i % 2
        win = POOLW[g]
        pp = np.zeros((128, 40), np.float32)
        sl = slice(128 * i, 128 * (i + 1))
        pp[:, 0:4] = inp['lru_conv_w'][l][:, sl].T
        pp[:, 4] = inp['lru_conv_b'][l][sl]
        pp[:, 5] = inp['lru_br'][l][sl]
        pp[:, 6] = inp['lru_bi'][l][sl]
        pp[:, 7] = inp['lru_lambda'][l][sl]
        cw = np.zeros(4, np.float32)
        cw[g] = 1.0 / win
        pp[:, 8:12] = cw
        osl = slice(256 * g + 128 * half, 256 * g + 128 * half + 128)
        pp[:, 12] = inp['pool_b'][l][osl]
        pp[:, 13] = inp['pool_scale'][l][osl]
        t = np.arange(16)
        pp[:, 14:30] = (win / np.minimum(t + 1, win)).astype(np.float32)
        pp[:, 30] = np.tile(inp['q_norm_g'][l], 2)
        pp[:, 31] = np.tile(inp['k_norm_g'][l], 2)
        pp[:, 32] = invf
        pp[:, 33] = sgn
        cvp = np.zeros((128, 8, 34), np.float32)
        cvp[:, :, 0:31] = inp['cv_dw_w'][l].T.reshape(8, 128, 31).transpose(1, 0, 2)
        cvp[:, :, 31] = inp['cv_dw_b'][l].reshape(8, 128).T
        cvp[:, :, 32] = inp['cv_ln_g'][l].reshape(8, 128).T
        cvp[:, :, 33] = inp['cv_ln_b'][l].reshape(8, 128).T
        b, seg = i // 4, i % 4
        cv = np.zeros((2, 1024, 30 + TS), np.float32)
        lo = seg * TS
        for w_, off in enumerate((6144, 7168)):
            src = projT[off:off + 1024, b * S_: (b + 1) * S_]
            if lo >= 30:
                cv[w_] = src[:, lo - 30: lo + TS]
            else:
                cv[w_, :, 30:] = src[:, lo: lo + TS]
        lamv = np.concatenate([inp['lam_q1'][l], inp['lam_k1'][l], inp['lam_q2'][l], inp['lam_k2'][l]])
        m = dict(consts)
        m.update(dict(
            lx=np.ascontiguousarray(projT[128 * i:128 * (i + 1)]),
            lg=np.ascontiguousarray(projT[1024 + 128 * i:1024 + 128 * (i + 1)]),
            px=np.ascontiguousarray(projT[2048 + 256 * g:2048 + 256 * (g + 1)]).reshape(2, 128, -1),
            q=np.ascontiguousarray(projT[3072 + 128 * i:3072 + 128 * (i + 1)]),
            k=np.ascontiguousarray(projT[4096 + 128 * i:4096 + 128 * (i + 1)]),
            v=np.ascontiguousarray(projT[5120 + 128 * i:5120 + 128 * (i + 1)]),
            cval=cv[0].reshape(8, 128, -1), cgate=cv[1].reshape(8, 128, -1), pp=pp, cvp=cvp,
            wr=np.ascontiguousarray(inp['lru_wr'][l][i]), wi=np.ascontiguousarray(inp['lru_wi'][l][i]),
            pw=np.ascontiguousarray(inp['pool_w'][l][g][:, 128 * half:128 * (half + 1)]).reshape(2, 128, 128),
            pos=np.ascontiguousarray(inp['positions'][:, :S_]).astype(np.int32),
            lamv=np.ascontiguousarray(np.broadcast_to(lamv, (128, 256))).astype(np.float32),
            sgb=np.ascontiguousarray(np.broadcast_to(inp['subln_g'][l], (128, 128))).astype(np.float32),
            linit=np.ascontiguousarray(np.broadcast_to(np.array([lambda_init, 1 - lambda_init], np.float32), (128, 2)))))
        maps.append(m)
    return maps


def _lb_gather(results, S_):
    TS = S_ // 4
    yT = np.zeros((4096, 2 * S_), BF)
    for i in range(8):
        r = results[i]
        yT[128 * i:128 * (i + 1)] = r['ya']
        g, half = i // 2, i % 2
        yT[1024 + 256 * g + 128 * half: 1024 + 256 * g + 128 * half + 128] = r['yb']
        yT[2048 + 128 * i: 2048 + 128 * (i + 1)] = r['yc']
        b, seg = i // 4, i % 4
        yT[3072:4096, b * S_ + seg * TS: b * S_ + (seg + 1) * TS] = np.asarray(r['yd']).reshape(1024, TS)
    return yT


_PROGS = {}


def _prog(name, fn):
    if name not in _PROGS:
        _PROGS[name] = fn()
    return _PROGS[name]


import sys as _sys
import time as _time
_T0 = [None]


def _log(msg):
    if _T0[0] is None:
        _T0[0] = _time.time()
    print("[kernel %.0fs] %s" % (_time.time() - _T0[0], msg), file=_sys.stderr, flush=True)


def _run(nc, maps):
    _log("launch start")
    r = run_bass_kernel_spmd(nc, maps, core_ids=list(range(NCORES))).results
    _log("launch done")
    return r


def kernel(**inp):
    inp = {k: np.asarray(v) for k, v in inp.items()}
    x = np.ascontiguousarray(inp['x'], dtype=np.float32).reshape(B * SEQ, D)
    c = np.ascontiguousarray(inp['c'], dtype=np.float32)
    ident = np.eye(128, dtype=np.float32)
    identb = ident.astype(BF)
    sel = np.zeros((32, 32, 128), np.float32)
    for e in range(32):
        sel[e, e, :] = 1.0
    maps = []
    for i in range(NCORES):
        maps.append({"c": c, "adaw": np.ascontiguousarray(inp['ada_w'][:, :, 3072 * i:3072 * (i + 1)]),
                     "adab": np.ascontiguousarray(inp['ada_b'][:, None, 3072 * i:3072 * (i + 1)]), "ident": ident})
    res = _run(_prog("L0", build_L0), maps)
    modfm = np.zeros((2, 2, 128, 192), np.float32)
    for i in range(NCORES):
        modfm[:, :, :, i * 24:(i + 1) * 24] = np.asarray(res[i]["modT"]).transpose(0, 3, 1, 2)
    del maps, res
    for l in range(2):
        win = np.ascontiguousarray(inp['w_in'][l][:, :NMIX])
        gfm = np.ascontiguousarray(inp['norm1_g'][l].reshape(32, 128).T)
        maps = [{"x": x[i * TPC:(i + 1) * TPC], "modfm": modfm[l, i // 4], "gfm": gfm, "win": win, "identb": identb}
                for i in range(NCORES)]
        res = _run(_prog("LA", build_LA), maps)
        projT = np.concatenate([np.asarray(r["projT"]) for r in res], axis=1)
        hT = [np.asarray(r["hT"]) for r in res]
        del maps, res, win
        res = _run(_prog("LB", build_LB), _lb_maps(projT, inp, l, SEQ))
        yT = _lb_gather(res, SEQ)
        del res, projT
        wg = np.ascontiguousarray(inp['w_in'][l][:, NMIX:])
        wrt = np.ascontiguousarray(np.concatenate([inp['router_g_w'][l], inp['router_e_w'][l]], axis=1))
        rb = np.ascontiguousarray(np.concatenate([inp['router_g_b'][l], inp['router_e_b'][l]])[None]).astype(np.float32)
        g2fm = np.ascontiguousarray(inp['norm2_g'][l].reshape(32, 128).T)
        bgfm = np.ascontiguousarray(inp['b_gate'][l].reshape(4, 32, 128).transpose(2, 0, 1))
        maps = [dict(yT=np.ascontiguousarray(yT[:, i * TPC:(i + 1) * TPC]), hT=hT[i], x=x[i * TPC:(i + 1) * TPC],
                     modfm=modfm[l, i // 4], g2fm=g2fm, bgfm=bgfm, wg=wg, wbr=inp['w_branch'][l], wo=inp['w_out'][l],
                     wrt=wrt, rb=rb, ident=ident) for i in range(NCORES)]
        res = _run(_prog("LC", build_LC), maps)
        xmid = [np.asarray(r["xmid"]) for r in res]
        h2T = [np.asarray(r["h2T"]) for r in res]
        comb = [np.asarray(r["comb"]) for r in res]
        del maps, res, wg, yT, hT
        maps = []
        for i in range(NCORES):
            g2row = modfm[l, i // 4][:, 160:192].T.reshape(-1)
            maps.append(dict(h2T=h2T[i], combT=np.ascontiguousarray(comb[i].T), xmid=xmid[i],
                             g2b=np.ascontiguousarray(np.broadcast_to(g2row, (128, D))).astype(np.float32),
                             w1=inp['moe_w1'][l], w3=inp['moe_w3'][l], w2=inp['moe_w2'][l]))
        res = _run(_prog("LD", build_LD), maps)
        x = np.concatenate([np.asarray(r["xout"]) for r in res], axis=0)
        del maps, res, xmid, h2T, comb
    return x.reshape(B, SEQ, D).astype(np.float32)
```
